# Optimizing a Trainium2 kernel written in Bass

```python
import math
import jax
import jax.numpy as jnp
from jax import lax
import numpy as np


D_MODEL = 1024
BATCH = 16
SEQ = 2048
DEPTH = 4

CTX_LEN = 256
GRID_W = 64
EPS = 1e-6
NEG = -1e30
LB_FLOOR = 1e-30
ML_HEADS = 4
ML_HD = 96
ML_W = ML_HEADS * ML_HD
ML_CHUNK = 64
M_INIT = -1e30
HG_HEADS = 4
HG_DV = 64
HG_DK = 128
HG_W = HG_HEADS * HG_DV
HG_FW = HG_HEADS * HG_DK
HG_CHUNK = 32
DA_HEADS = 4
DA_HD = 48
DA_VD = 2 * DA_HD
DA_W = DA_HEADS * DA_VD
Q_BLOCK = 128
ROPE_BASE = 10000.0

D_MIX = ML_W + HG_W + DA_W
D_FF = ((8 * D_MODEL + 3 * 256 - 1) // (3 * 256)) * 256

IN_SIZES = (ML_W, ML_W, ML_W, ML_W, 2 * ML_HEADS, 2 * ML_HEADS,
            HG_FW, 2 * HG_FW, HG_W, HG_W,
            2 * DA_HEADS * DA_HD, 2 * DA_HEADS * DA_HD, DA_W)
D_IN = sum(IN_SIZES)

kernel_name = 'hybrid_mlstm_hgrn2_diffattn_dit_block'


def split_cols(p):
    idx = np.cumsum(IN_SIZES)[:-1].tolist()
    return jnp.split(p, idx, axis=-1)


def rmsnorm(x, g):
    xf = x.astype(jnp.float32)
    y = xf * lax.rsqrt(jnp.mean(xf * xf, axis=-1, keepdims=True) + EPS)
    return (y * g.astype(jnp.float32)).astype(x.dtype)


def head_rmsnorm(h, gain):
    d = h.shape[-1]
    y = h * lax.rsqrt(jnp.mean(h * h, axis=-1, keepdims=True) + EPS)
    return y * gain.astype(jnp.float32).reshape(-1, 1, d)


def to_heads(t, h):
    b, n, _ = t.shape
    return t.reshape(b, n, h, -1).transpose(0, 2, 1, 3)


def from_heads(t):
    b, h, n, d = t.shape
    return t.transpose(0, 2, 1, 3).reshape(b, n, h * d)


def to_chunks(t, size):
    b, h, n = t.shape[:3]
    t = t.reshape(b, h, n // size, size, *t.shape[3:])
    return jnp.moveaxis(t, 2, 0)


def from_chunks(t):
    t = jnp.moveaxis(t, 0, 2)
    return t.reshape(t.shape[0], t.shape[1], -1, *t.shape[4:])


def _flip(t, rev):
    return jnp.flip(t, axis=2) if rev else t


def prefix_bidirectional(scan_fn, ctx_shared, ctx_dir, lat_shared, lat_dir, state0):
    out_c = None
    out_x = None
    for rev in (0, 1):
        oc, st = scan_fn(*[_flip(t, rev) for t in ctx_shared + ctx_dir[rev]], state0)
        ox, _ = scan_fn(*[_flip(t, rev) for t in lat_shared + lat_dir[rev]], st)
        oc = _flip(oc, rev)
        ox = _flip(ox, rev)
        out_c = oc if out_c is None else out_c + oc
        out_x = ox if out_x is None else out_x + ox
    return out_c, out_x


def mlstm_scan(q, k, v, ig, lf, state):
    size = ML_CHUNK
    mask = jnp.tril(jnp.ones((size, size), bool))

    def step(carry, inp):
        S, nv, m = carry
        qc, kc, vc, ic, fc = inp
        b = jnp.cumsum(fc, axis=-1)
        a = b + m[..., None]
        dmat = jnp.where(mask, b[..., :, None] - b[..., None, :] + ic[..., None, :], NEG)
        mt = jnp.maximum(a, jnp.max(dmat, axis=-1))
        w_inter = jnp.exp(a - mt)
        qk = jnp.einsum('bhtd,bhsd->bhts', qc, kc) * jnp.exp(dmat - mt[..., None])
        num = jnp.einsum('bhts,bhsv->bhtv', qk, vc) + w_inter[..., None] * jnp.einsum('bhtd,bhdv->bhtv', qc, S)
        den = jnp.sum(qk, axis=-1) + w_inter * jnp.einsum('bhtd,bhd->bht', qc, nv)
        h = num / jnp.maximum(jnp.abs(den), jnp.exp(-mt))[..., None]
        m_new = mt[..., -1]
        decay = jnp.exp(b[..., -1] + m - m_new)
        wk = jnp.exp(b[..., -1:] - b + ic - m_new[..., None])
        S_new = decay[..., None, None] * S + jnp.einsum('bhs,bhsd,bhsv->bhdv', wk, kc, vc)
        n_new = decay[..., None] * nv + jnp.einsum('bhs,bhsd->bhd', wk, kc)
        return (S_new, n_new, m_new), h

    xs = tuple(to_chunks(t, size) for t in (q, k, v, ig, lf))
    state, h = lax.scan(step, state, xs)
    return from_chunks(h), state


def mlstm_prep(parts, f_bias):
    q, k, v, _, i, f = parts
    b, n, _ = q.shape
    f32 = jnp.float32
    qh = to_heads(q, ML_HEADS).astype(f32)
    kh = to_heads(k, ML_HEADS).astype(f32) * (ML_HD ** -0.5)
    vh = to_heads(v, ML_HEADS).astype(f32)
    gi = i.astype(f32).reshape(b, n, 2, ML_HEADS).transpose(0, 2, 3, 1)
    gf = jax.nn.log_sigmoid((f + f_bias).astype(f32)).reshape(b, n, 2, ML_HEADS).transpose(0, 2, 3, 1)
    return (qh, kh, vh), ((gi[:, 0], gf[:, 0]), (gi[:, 1], gf[:, 1]))


def mlstm_mixer(pc, px, f_bias, gain, with_ctx):
    sc, dc = mlstm_prep(pc, f_bias)
    sx, dx = mlstm_prep(px, f_bias)
    b = sx[0].shape[0]
    state0 = (jnp.zeros((b, ML_HEADS, ML_HD, ML_HD), jnp.float32),
              jnp.zeros((b, ML_HEADS, ML_HD), jnp.float32),
              jnp.full((b, ML_HEADS), M_INIT, jnp.float32))
    hc, hx = prefix_bidirectional(mlstm_scan, sc, dc, sx, dx, state0)

    def finish(h, o):
        y = from_heads(head_rmsnorm(h, gain)) * jax.nn.sigmoid(o.astype(jnp.float32))
        return y.astype(o.dtype)

    yc = finish(hc, pc[3]) if with_ctx else None
    return yc, finish(hx, px[3])


def hgrn2_scan(q, v, k, lf, S):
    size = HG_CHUNK
    mask = jnp.tril(jnp.ones((size, size), bool))[..., None]

    def step(S, inp):
        qc, vc, kc, fc = inp
        bc = jnp.cumsum(fc, axis=2)
        rel = jnp.where(mask, bc[:, :, :, None] - bc[:, :, None], NEG)
        att = jnp.einsum('bhtk,bhtsk->bhts', qc, jnp.exp(rel) * kc[:, :, None])
        o = jnp.einsum('bhts,bhsv->bhtv', att, vc) + jnp.einsum('bhtk,bhkv->bhtv', qc * jnp.exp(bc), S)
        last = bc[:, :, -1:]
        S_new = jnp.exp(last[:, :, 0])[..., None] * S + jnp.einsum('bhsk,bhsv->bhkv', kc * jnp.exp(last - bc), vc)
        return S_new, o

    xs = tuple(to_chunks(t, size) for t in (q, v, k, lf))
    S, o = lax.scan(step, S, xs)
    return from_chunks(o), S


def hgrn2_prep(parts, lb):
    q, f, i, _ = parts
    b, n, _ = q.shape
    f32 = jnp.float32
    qh = to_heads(jax.nn.silu(q.astype(f32)), HG_HEADS)
    vh = to_heads(i.astype(f32), HG_HEADS)
    log_lb = jnp.log(jnp.maximum(lb, LB_FLOOR))
    logf = jnp.logaddexp(log_lb, jnp.log1p(-lb) + jax.nn.log_sigmoid(f.astype(f32)))
    logf = logf.reshape(b, n, 2, HG_HEADS, HG_DK).transpose(0, 2, 3, 1, 4)
    kk = -jnp.expm1(logf)
    return (qh, vh), ((kk[:, 0], logf[:, 0]), (kk[:, 1], logf[:, 1]))


def hgrn2_mixer(pc, px, lb, gain, with_ctx):
    sc, dc = hgrn2_prep(pc, lb)
    sx, dx = hgrn2_prep(px, lb)
    b = sx[0].shape[0]
    state0 = jnp.zeros((b, HG_HEADS, HG_DK, HG_DV), jnp.float32)
    oc, ox = prefix_bidirectional(hgrn2_scan, sc, dc, sx, dx, state0)

    def finish(o, g):
        y = from_heads(head_rmsnorm(o, gain)) * jax.nn.silu(g.astype(jnp.float32))
        return y.astype(g.dtype)

    yc = finish(oc, pc[3]) if with_ctx else None
    return yc, finish(ox, px[3])


def axial_angles(n, d):
    rows = n // GRID_W
    row = jnp.repeat(jnp.arange(rows), GRID_W).astype(jnp.float32)
    col = jnp.tile(jnp.arange(GRID_W), rows).astype(jnp.float32)
    half = d // 2
    inv = ROPE_BASE ** (-jnp.arange(0, half, 2, dtype=jnp.float32) / half)
    return row[:, None] * inv, col[:, None] * inv


def rope_half(x, ang):
    x1, x2 = jnp.split(x.astype(jnp.float32), 2, axis=-1)
    c, s = jnp.cos(ang), jnp.sin(ang)
    return jnp.concatenate([x1 * c - x2 * s, x1 * s + x2 * c], axis=-1)


def axial_rope(x, ang_r, ang_c):
    xr, xc = jnp.split(x, 2, axis=-1)
    return jnp.concatenate([rope_half(xr, ang_r), rope_half(xc, ang_c)], axis=-1).astype(x.dtype)


def diff_heads(t):
    b, n, _ = t.shape
    return t.reshape(b, n, DA_HEADS, 2, DA_HD).transpose(0, 2, 3, 1, 4)


def diff_attn(q, k, v, lam):
    s = jnp.einsum('bhjqd,bhjkd->bhjqk', q, k).astype(jnp.float32) * (DA_HD ** -0.5)
    p = jax.nn.softmax(s, axis=-1)
    a = p[:, :, 0] - lam * p[:, :, 1]
    return jnp.einsum('bhqk,bhkv->bhqv', a.astype(v.dtype), v)


def blocked_diff_attn(q, k, v, lam):
    b, h, _, n, d = q.shape
    qb = jnp.moveaxis(q.reshape(b, h, 2, n // Q_BLOCK, Q_BLOCK, d), 3, 0)
    ob = lax.map(lambda qq: diff_attn(qq, k, v, lam), qb)
    return jnp.moveaxis(ob, 0, 2).reshape(b, h, n, -1)


def diff_mixer(pc, px, lam_vec, gain, lam_init, ang_r, ang_c, with_ctx):
    qc, kc, vc = diff_heads(pc[0]), diff_heads(pc[1]), to_heads(pc[2], DA_HEADS)
    qx = axial_rope(diff_heads(px[0]), ang_r, ang_c)
    kx = axial_rope(diff_heads(px[1]), ang_r, ang_c)
    vx = to_heads(px[2], DA_HEADS)
    lv = lam_vec.astype(jnp.float32)
    lam = jnp.exp(jnp.sum(lv[0] * lv[1])) - jnp.exp(jnp.sum(lv[2] * lv[3])) + lam_init
    k_all = jnp.concatenate([kc, kx], axis=3)
    v_all = jnp.concatenate([vc, vx], axis=2)
    ox = blocked_diff_attn(qx, k_all, v_all, lam)

    def finish(o, dt):
        return (from_heads(head_rmsnorm(o.astype(jnp.float32), gain)) * (1.0 - lam_init)).astype(dt)

    yc = finish(diff_attn(qc, kc, vc, lam), pc[2].dtype) if with_ctx else None
    return yc, finish(ox, px[2].dtype)


def token_mixer(hc, hx, w_in, b_in, w_out, ml_fb, ml_g, lb, hg_g, da_lam, da_g, lam_init, ang_r, ang_c, with_ctx):
    pc = split_cols(hc @ w_in + b_in)
    px = split_cols(hx @ w_in + b_in)
    mc, mx = mlstm_mixer(pc[0:6], px[0:6], ml_fb, ml_g, with_ctx)
    gc, gx = hgrn2_mixer(pc[6:10], px[6:10], lb, hg_g, with_ctx)
    ac, ax = diff_mixer(pc[10:13], px[10:13], da_lam, da_g, lam_init, ang_r, ang_c, with_ctx)
    yx = jnp.concatenate([mx, gx, ax], axis=-1) @ w_out
    yc = jnp.concatenate([mc, gc, ac], axis=-1) @ w_out if with_ctx else None
    return yc, yx


def swiglu(h, wg, wu, wd):
    return (jax.nn.silu(h @ wg) * (h @ wu)) @ wd


def setup_inputs(seed: int = 0) -> dict:
    key = jax.random.key(seed)
    ks = jax.random.split(key, 24)
    f32 = jnp.float32
    L = DEPTH
    D = D_MODEL

    def nrm(k, shape, s):
        return s * jax.random.normal(k, shape, f32)

    return {
        'x': nrm(ks[0], (BATCH, SEQ, D), 1.0),
        'c': nrm(ks[1], (BATCH, D), 1.0),
        'ctx': nrm(ks[2], (BATCH, CTX_LEN, D), 1.0),
        'c_ctx': nrm(ks[3], (D,), 1.0),
        'w_ada': nrm(ks[4], (L, D, 6 * D), 0.5 * D ** -0.5),
        'b_ada': nrm(ks[5], (L, 6 * D), 0.02),
        'g_pre_mix': 1.0 + nrm(ks[6], (L, D), 0.1),
        'g_post_mix': 1.0 + nrm(ks[7], (L, D), 0.1),
        'g_pre_ffn': 1.0 + nrm(ks[8], (L, D), 0.1),
        'g_post_ffn': 1.0 + nrm(ks[9], (L, D), 0.1),
        'w_in': nrm(ks[10], (L, D, D_IN), D ** -0.5),
        'b_in': nrm(ks[11], (L, D_IN), 0.02),
        'w_out': nrm(ks[12], (L, D_MIX, D), D_MIX ** -0.5),
        'ml_f_bias': jnp.linspace(3.0, 6.0, 2 * ML_HEADS, dtype=f32)[None] + nrm(ks[13], (L, 2 * ML_HEADS), 0.1),
        'ml_norm': 1.0 + nrm(ks[14], (L, ML_W), 0.1),
        'hg_lb': nrm(ks[15], (L, 2 * HG_FW), 1.0),
        'hg_norm': 1.0 + nrm(ks[16], (L, HG_W), 0.1),
        'da_lambda': nrm(ks[17], (L, 4, DA_HD), 0.1),
        'da_norm': 1.0 + nrm(ks[18], (L, DA_VD), 0.1),
        'w_ffn_gate': nrm(ks[19], (L, D, D_FF), D ** -0.5),
        'w_ffn_up': nrm(ks[20], (L, D, D_FF), D ** -0.5),
        'w_ffn_down': nrm(ks[21], (L, D_FF, D), D_FF ** -0.5),
    }


def reference(x, c, ctx, c_ctx, w_ada, b_ada, g_pre_mix, g_post_mix, g_pre_ffn, g_post_ffn,
              w_in, b_in, w_out, ml_f_bias, ml_norm, hg_lb, hg_norm, da_lambda, da_norm,
              w_ffn_gate, w_ffn_up, w_ffn_down):
    n = x.shape[1]
    ang_r, ang_c = axial_angles(n, DA_HD)
    sm = jax.nn.softmax(hg_lb.astype(jnp.float32), axis=0)
    lbs = jnp.cumsum(sm, axis=0) - sm[0:1]
    sc_ = jax.nn.silu(c)
    scc = jax.nn.silu(c_ctx)
    for l in range(DEPTH):
        with_ctx = l < DEPTH - 1
        lam_init = 0.8 - 0.6 * math.exp(-0.3 * l)
        mx_ = (sc_ @ w_ada[l] + b_ada[l])[:, None, :]
        mc_ = scc @ w_ada[l] + b_ada[l]
        sx1, ax1, gx1, sx2, ax2, gx2 = jnp.split(mx_, 6, axis=-1)
        sc1, ac1, gc1, sc2, ac2, gc2 = jnp.split(mc_, 6, axis=-1)
        hx = rmsnorm(x, g_pre_mix[l]) * (1.0 + ax1) + sx1
        hc = rmsnorm(ctx, g_pre_mix[l]) * (1.0 + ac1) + sc1
        yc, yx = token_mixer(hc, hx, w_in[l], b_in[l], w_out[l], ml_f_bias[l], ml_norm[l], lbs[l],
                             hg_norm[l], da_lambda[l], da_norm[l], lam_init, ang_r, ang_c, with_ctx)
        x = x + gx1 * rmsnorm(yx, g_post_mix[l])
        hx = rmsnorm(x, g_pre_ffn[l]) * (1.0 + ax2) + sx2
        x = x + gx2 * rmsnorm(swiglu(hx, w_ffn_gate[l], w_ffn_up[l], w_ffn_down[l]), g_post_ffn[l])
        if with_ctx:
            ctx = ctx + gc1 * rmsnorm(yc, g_post_mix[l])
            hcf = rmsnorm(ctx, g_pre_ffn[l]) * (1.0 + ac2) + sc2
            ctx = ctx + gc2 * rmsnorm(swiglu(hcf, w_ffn_gate[l], w_ffn_up[l], w_ffn_down[l]), g_post_ffn[l])
    return x
```

```python
import numpy as np
import concourse.bass as bass
import concourse.mybir as mybir

F32 = mybir.dt.float32
BF16 = mybir.dt.bfloat16
AF = mybir.ActivationFunctionType
ALU = mybir.AluOpType

SEM_LIM = 30000
N_DMA_SEMS = 40


class View:
    __slots__ = ("t", "ap", "p0", "p1", "f0", "f1")

    def __init__(self, t, ap, p0, p1, f0, f1):
        self.t, self.ap, self.p0, self.p1, self.f0, self.f1 = t, ap, p0, p1, f0, f1


class Tile:
    def __init__(self, kb, h, P, F, dt, name):
        self.kb, self.h, self.P, self.F, self.dt, self.name = kb, h, P, F, dt, name
        self.recs = []

    def v(self, p0, n, off, *dims):
        if not dims:
            raise ValueError
        ap = bass.AP(self.h, p0 * self.F + off, [[self.F, n]] + [[s, c] for s, c in dims])
        hi = off + sum((c - 1) * s for s, c in dims) + 1
        assert 0 <= off and hi <= self.F and p0 + n <= self.P, (self.name, p0, n, off, dims, self.F)
        return View(self, ap, p0, p0 + n, off, hi)

    def c(self, p0, n, off, cnt):
        return self.v(p0, n, off, (1, cnt))

    def track(self, v, is_write, opid, eng):
        deps = set()
        keep = []
        for r in self.recs:
            ov = not (r[1] <= v.p0 or v.p1 <= r[0] or r[3] <= v.f0 or v.f1 <= r[2])
            if not ov:
                keep.append(r)
                continue
            contained = (v.p0 <= r[0] and r[1] <= v.p1 and v.f0 <= r[2] and r[3] <= v.f1)
            if is_write:
                deps.add(r[4])
                if not contained:
                    keep.append(r)
            else:
                if r[5]:
                    deps.add(r[4])
                    keep.append(r)
                else:
                    if not (r[6] == eng and contained):
                        keep.append(r)
        keep.append((v.p0, v.p1, v.f0, v.f1, opid, is_write, eng))
        self.recs = keep
        return deps


class Op:
    __slots__ = ("eng", "emit", "deps", "waits", "signal", "local", "is_dma", "dsem", "dval", "sigsem", "sigval")

    def __init__(self, eng, emit, deps, is_dma=False):
        self.eng, self.emit, self.deps, self.is_dma = eng, emit, deps, is_dma
        self.waits = []
        self.signal = False
        self.local = -1
        self.dsem = -1
        self.dval = 0
        self.sigsem = None
        self.sigval = 0


ENGS = ("pe", "act", "dve", "pool", "sp")


class KB:
    def __init__(self, nc):
        self.nc = nc
        self.ops = []
        self.stack = []
        self.dma_rr = 0
        self.dma_last = [None] * N_DMA_SEMS
        self.dma_cnt = [0] * N_DMA_SEMS
        self.out_dmas = []

    def sb(self, name, P, F, dt):
        g = self.nc.sbuf_tensor(name, [P, F], dt)
        h = g.__enter__()
        self.stack.append(g)
        return Tile(self, h, P, F, dt, name)

    def ps(self, name, P, F, dt):
        g = self.nc.psum_tensor(name, [P, F], dt)
        h = g.__enter__()
        self.stack.append(g)
        return Tile(self, h, P, F, dt, name)

    def dr(self, name, R, C, dt, kind="Internal"):
        h = self.nc.dram_tensor(name, [R, C], dt, kind=kind)
        return Tile(self, h, R, C, dt, name)

    def op(self, eng, emit, reads, writes, is_dma=False):
        opid = len(self.ops)
        deps = set()
        for v in reads:
            if isinstance(v, View):
                deps |= v.t.track(v, False, opid, eng)
        for v in writes:
            if isinstance(v, View):
                deps |= v.t.track(v, True, opid, eng)
        deps.discard(opid)
        o = Op(eng, emit, deps, is_dma)
        if is_dma:
            s = self.dma_rr
            self.dma_rr = (self.dma_rr + 1) % N_DMA_SEMS
            if self.dma_last[s] is not None:
                o.deps.add(self.dma_last[s])
            self.dma_last[s] = opid
            self.dma_cnt[s] += 1
            o.dsem = s
            o.dval = 16 * self.dma_cnt[s]
        self.ops.append(o)
        return opid

    @staticmethod
    def _ap(x):
        return x.ap if isinstance(x, View) else x

    def mm(self, out, lhsT, rhs, start=True, stop=True, **kw):
        o, l, r = out.ap, lhsT.ap, rhs.ap
        return self.op("pe", lambda e: e.matmul(o, l, r, start=start, stop=stop, **kw), [lhsT, rhs], [out])

    def tr(self, out, in_, ident):
        o, i, d = out.ap, in_.ap, ident.ap
        return self.op("pe", lambda e: e.transpose(o, i, d), [in_, ident], [out])

    def act(self, out, in_, func, bias=0.0, scale=1.0, accum=None):
        o, i = out.ap, in_.ap
        b = self._ap(bias)
        s = self._ap(scale)
        reads = [in_] + [x for x in (bias, scale) if isinstance(x, View)]
        writes = [out] + ([accum] if accum is not None else [])
        if accum is not None:
            a = accum.ap
            f = lambda e: e.activation(o, i, func, bias=b, scale=s, accum_out=a)
        else:
            f = lambda e: e.activation(o, i, func, bias=b, scale=s)
        return self.op("act", f, reads, writes)

    def ts(self, out, in0, s1, s2, op0, op1=None, eng="dve"):
        o, i = out.ap, in0.ap
        a, b = self._ap(s1), self._ap(s2)
        reads = [in0] + [x for x in (s1, s2) if isinstance(x, View)]
        if op1 is None:
            f = lambda e: e.tensor_scalar(o, i, a, None, op0)
        else:
            f = lambda e: e.tensor_scalar(o, i, a, b, op0, op1)
        return self.op(eng, f, reads, [out])

    def tt(self, out, in0, in1, op, eng="dve"):
        o, a, b = out.ap, in0.ap, in1.ap
        return self.op(eng, lambda e: e.tensor_tensor(o, a, b, op), [in0, in1], [out])

    def stt(self, out, in0, scalar, in1, op0, op1, accum=None):
        o, a, b = out.ap, in0.ap, in1.ap
        s = self._ap(scalar)
        reads = [in0, in1] + ([scalar] if isinstance(scalar, View) else [])
        writes = [out] + ([accum] if accum is not None else [])
        if accum is not None:
            ac = accum.ap
            f = lambda e: e.scalar_tensor_tensor(o, a, s, b, op0, op1, accum_out=ac)
        else:
            f = lambda e: e.scalar_tensor_tensor(o, a, s, b, op0, op1)
        return self.op("dve", f, reads, writes)

    def copy(self, out, in_, eng="dve"):
        o, i = out.ap, in_.ap
        if eng == "act":
            return self.op("act", lambda e: e.copy(o, i), [in_], [out])
        return self.op(eng, lambda e: e.tensor_copy(o, i), [in_], [out])

    def memset(self, out, val, eng="dve"):
        o = out.ap
        return self.op(eng, lambda e: e.memset(o, val), [], [out])

    def recip(self, out, in_):
        o, i = out.ap, in_.ap
        return self.op("dve", lambda e: e.reciprocal(o, i), [in_], [out])

    def scan(self, out, d0, d1, init, op0, op1):
        o, a, b = out.ap, d0.ap, d1.ap
        return self.op("dve", lambda e: e.tensor_tensor_scan(o, a, b, init, op0, op1), [d0, d1], [out])

    def dma(self, q, out, in_, is_output=False):
        o, i = self._ap(out), self._ap(in_)
        opid = self.op(q, lambda e: e.dma_start(out=o, in_=i), [in_], [out], is_dma=True)
        if is_output:
            self.out_dmas.append(opid)
        return opid

    def finish(self):
        nc = self.nc
        ops = self.ops
        fin = Op("sp", None, set(self.out_dmas))
        ops.append(fin)
        per = {e: [] for e in ENGS}
        for o in ops:
            o.local = len(per[o.eng])
            per[o.eng].append(o)
        seen = {e: {} for e in ENGS}
        for o in ops:
            sd = seen[o.eng]
            best = {}
            for d in o.deps:
                p = ops[d]
                if p.is_dma:
                    key, idx = ("d", p.dsem), p.dval
                else:
                    if p.eng == "pe" and o.eng == "pe":
                        continue
                    key, idx = ("e", p.eng), p.local
                if key not in best or best[key][0] < idx:
                    best[key] = (idx, d)
            for key, (idx, d) in best.items():
                if sd.get(key, -1) >= idx:
                    continue
                sd[key] = idx
                o.waits.append(d)
                ops[d].signal = True
        nsig = {e: sum(1 for o in per[e] if o.signal and not o.is_dma) for e in ENGS}
        sems = {}
        for e in ENGS:
            k = nsig[e] // SEM_LIM + 1
            sems[e] = []
            for j in range(k):
                g = nc.semaphore(f"s_{e}_{j}")
                sems[e].append(g.__enter__())
                self.stack.append(g)
        dsems = []
        for j in range(N_DMA_SEMS):
            g = nc.semaphore(f"s_dma_{j}")
            dsems.append(g.__enter__())
            self.stack.append(g)
        for e in ENGS:
            c = 0
            for o in per[e]:
                if o.signal and not o.is_dma:
                    o.sigsem = sems[e][c // SEM_LIM]
                    o.sigval = c % SEM_LIM + 1
                    c += 1

        def run(e_name):
            def f(e):
                for o in per[e_name]:
                    for d in o.waits:
                        p = ops[d]
                        if p.is_dma:
                            e.wait_ge(dsems[p.dsem], p.dval)
                        else:
                            e.wait_ge(p.sigsem, p.sigval)
                    if o.emit is None:
                        continue
                    ins = o.emit(e)
                    if o.is_dma:
                        ins.then_inc(dsems[o.dsem], 16)
                    elif o.signal:
                        ins.then_inc(o.sigsem, 1)
            return f

        g = nc.Block()
        block = g.__enter__()
        block.tensor(run("pe"))
        block.scalar(run("act"))
        block.vector(run("dve"))
        block.gpsimd(run("pool"))
        block.sync(run("sp"))
        g.__exit__(None, None, None)
        for g in reversed(self.stack):
            g.__exit__(None, None, None)
        self.stats = {e: len(per[e]) for e in ENGS}
        self.stats["signals"] = nsig

from concourse.bass_utils import run_bass_kernel_spmd
import math

D = 1024
KC = 8
DFF = 2816
FC = 22
EPS = 1e-6
OFF = dict(mq=0, mk=384, mv=768, mo=1152, mi=1536, mf=1544, hq=1552, hf=2064, hi=3088, hg=3344,
           dq=3600, dk=3984, dv=4368)
DIN = 4752
NCH = 44
NTM = 12
WSLOT = 2048


class Cfg:
    def __init__(s, N=2048, NC=256, L=4, NB=2, layer0=0, LTOT=4):
        s.N, s.NC, s.L, s.NB, s.layer0, s.LTOT = N, NC, L, NB, layer0, LTOT
        s.T = N + NC
        s.NT = s.T // 128
        s.groups = []
        t = 0
        while t < NC:
            n = min(512, NC - t)
            s.groups.append((t, n, True))
            t += n
        while t < s.T:
            n = min(512, s.T - t)
            s.groups.append((t, n, False))
            t += n


def fm_cols():
    cols = -np.ones((NCH, 128), np.int64)

    def partner(d):
        blk, i = divmod(d, 24)
        return blk * 24 + (i + 12 if i < 12 else i - 12)
    for h in range(4):
        for j, base in ((0, OFF['dq']), (2, OFF['dk'])):
            for m in range(2):
                for d in range(48):
                    cols[4 * h + j, 64 * m + d] = base + h * 96 + m * 48 + d
                    cols[4 * h + j + 1, 64 * m + d] = base + h * 96 + m * 48 + partner(d)
    for g in range(8):
        cols[16, g] = OFF['mi'] + g
        cols[17, g] = OFF['mf'] + g
    for h in range(4):
        for j in range(96):
            cols[18 + 3 * h, j] = OFF['mq'] + h * 96 + j
            cols[19 + 3 * h, j] = OFF['mk'] + h * 96 + j
            cols[20 + 3 * h, j] = OFF['mo'] + h * 96 + j
        for j in range(128):
            cols[30 + 3 * h, j] = OFF['hq'] + h * 128 + j
            cols[31 + 3 * h, j] = OFF['hf'] + h * 128 + j
            cols[32 + 3 * h, j] = OFF['hf'] + 512 + h * 128 + j
    for p in range(2):
        for j in range(128):
            cols[42 + p, j] = OFF['hg'] + p * 128 + j
    return cols


def tm_cols():
    cols = -np.ones((NTM, 128), np.int64)
    for h in range(4):
        for j in range(96):
            cols[h, j] = OFF['mv'] + h * 96 + j
            cols[8 + h, j] = OFF['dv'] + h * 96 + j
        for j in range(64):
            cols[4 + h, j] = OFF['hi'] + h * 64 + j
    return cols


def mix_rows():
    rows = -np.ones((10, 128), np.int64)
    for h in range(4):
        rows[h, :96] = np.arange(96) + h * 96
        rows[6 + h, :96] = 640 + np.arange(96) + h * 96
    for p in range(2):
        rows[4 + p, :] = 384 + p * 128 + np.arange(128)
    return rows


def rope_tables(cfg):
    T, NC = cfg.T, cfg.NC
    C = np.ones((128, T), np.float32)
    S = np.zeros((128, T), np.float32)
    inv = (10000.0 ** (-np.arange(0, 24, 2, dtype=np.float32) / 24)).astype(np.float32)
    t = np.arange(cfg.N)
    row = (t // 64).astype(np.float32)
    colp = (t % 64).astype(np.float32)
    for p in range(128):
        d = p % 64
        if d >= 48:
            continue
        blk, i = divmod(d, 24)
        j = i % 12
        pos = row if blk == 0 else colp
        ang = (pos * inv[j]).astype(np.float32)
        C[p, NC:] = np.cos(ang)
        S[p, NC:] = (-1.0 if i < 12 else 1.0) * np.sin(ang)
    return C, S


def host_consts(cfg):
    T = cfg.T
    s_ = np.arange(128)[:, None]
    t_ = np.arange(128)[None, :]
    mF = (s_ <= t_).astype(np.float32)
    mR = (s_ >= t_).astype(np.float32)
    same = (s_ // 32 == t_ // 32)
    m32F = (mF * same).astype(np.float32)
    m32R = (mR * same).astype(np.float32)
    ident = np.eye(128, dtype=np.float32)
    bmask = np.zeros((128, 4, 128), np.float32)
    for sc in range(4):
        bmask[32 * sc:32 * sc + 32, sc, :] = 1.0
    cst_bf = np.concatenate([ident, mF, mR, m32F, m32R, np.full((128, 128), 1.0 / 1024, np.float32), bmask.reshape(128, 512)], axis=1)
    e97 = np.zeros((128, 96), np.float32)
    e97[96, :] = 1.0
    ones96 = np.zeros((128, 96), np.float32)
    ones96[:96] = 1.0 / 96
    bd64 = np.zeros((128, 128), np.float32)
    bd64[:64, :64] = 1.0 / 64
    bd64[64:, 64:] = 1.0 / 64
    rst32 = np.ones((128, 512), np.float32)
    rst32[:, ::32] = 0.0
    c12 = np.zeros((128, 2), np.float32)
    c12[0:4, 0] = 1.0
    c12[4:8, 0] = -1.0
    c12[4:8, 1] = 1.0
    cst_f = np.concatenate([e97, ones96, bd64, rst32, c12], axis=1)
    C, S = rope_tables(cfg)
    return dict(cst_bf=cst_bf, cst_f=cst_f, ropeC=C, ropeS=S)


def prep_weights(inp, cfg):
    L0, L = cfg.layer0, cfg.L
    sl = slice(L0, L0 + L)
    out = {}
    w_ada = np.asarray(inp['w_ada'])[sl]
    out['wada'] = np.ascontiguousarray(
        w_ada.reshape(L, 8, 128, 12, 4, 128).transpose(0, 3, 2, 4, 1, 5).reshape(L, 12, 128, 4096))
    out['bada'] = np.ascontiguousarray(np.asarray(inp['b_ada'])[sl].reshape(L, 48, 128).transpose(2, 0, 1).reshape(128, L * 48))
    gv = np.stack([np.asarray(inp[k])[sl] for k in ('g_pre_mix', 'g_post_mix', 'g_pre_ffn', 'g_post_ffn')], 1)
    out['gvec'] = np.ascontiguousarray(gv.reshape(L, 4, 8, 128).transpose(3, 0, 1, 2).reshape(128, L * 32))
    w_in = np.asarray(inp['w_in'])[sl]
    b_in = np.asarray(inp['b_in'])[sl]
    wz = np.concatenate([w_in, np.zeros((L, D, 1), np.float32)], 2)
    bz = np.concatenate([b_in, np.zeros((L, 1), np.float32)], 1)
    fc_ = fm_cols()
    g = wz[:, :, fc_.reshape(-1)]
    out['win'] = np.ascontiguousarray(g.reshape(L, 8, 128, NCH, 128).transpose(0, 3, 2, 1, 4).reshape(L, NCH, 128, 1024))
    out['bin'] = np.ascontiguousarray(bz[:, fc_].transpose(2, 0, 1).reshape(128, L * NCH))
    tc_ = tm_cols()
    g = wz[:, :, tc_.reshape(-1)]
    out['wtm'] = np.ascontiguousarray(g.reshape(L, 8, 128, NTM, 128).transpose(0, 3, 2, 1, 4).reshape(L, NTM, 128, 1024))
    out['btm'] = np.ascontiguousarray(bz[:, tc_].reshape(1, L * NTM * 128))
    w_out = np.asarray(inp['w_out'])[sl]
    woz = np.concatenate([w_out, np.zeros((L, 1, D), np.float32)], 1)
    mr = mix_rows()
    g = woz[:, mr.reshape(-1), :]
    out['wout'] = np.ascontiguousarray(g.reshape(L, 10, 128, 8, 128).transpose(0, 3, 2, 1, 4).reshape(L, 8, 128, 1280))
    wg = np.asarray(inp['w_ffn_gate'])[sl].reshape(L, 8, 128, FC, 128)
    wu = np.asarray(inp['w_ffn_up'])[sl].reshape(L, 8, 128, FC, 128)
    gu = np.stack([wg, wu], 1)
    out['wgu'] = np.ascontiguousarray(gu.transpose(0, 4, 3, 1, 2, 5).reshape(L, FC, 128, 2048))
    wd = np.asarray(inp['w_ffn_down'])[sl].reshape(L, FC, 128, 8, 128)
    out['wd'] = np.ascontiguousarray(wd.transpose(0, 3, 2, 1, 4).reshape(L, 8, 128, FC * 128))
    mln = np.zeros((128, L * 4), np.float32)
    mln[:96] = np.asarray(inp['ml_norm'])[sl].reshape(L, 4, 96).transpose(2, 0, 1).reshape(96, L * 4)
    out['mlnorm'] = mln
    dan = np.zeros((128, L), np.float32)
    dan[:96] = np.asarray(inp['da_norm'])[sl].T
    out['danorm'] = dan
    out['hgnorm'] = np.ascontiguousarray(np.asarray(inp['hg_norm'])[sl].reshape(L, 2, 128).transpose(2, 0, 1).reshape(128, L * 2))
    out['hglb'] = np.ascontiguousarray(np.asarray(inp['hg_lb']).reshape(cfg.LTOT, 8, 128).transpose(2, 0, 1).reshape(128, cfg.LTOT * 8))
    out['dalam'] = np.ascontiguousarray(np.asarray(inp['da_lambda'])[sl].reshape(1, L * 192))
    mlfb = np.zeros((128, L), np.float32)
    mlfb[:8] = np.asarray(inp['ml_f_bias'])[sl].T
    out['mlfb'] = mlfb
    return out


def prep_core(inp, cfg, b0):
    NB = cfg.NB
    x = np.asarray(inp['x'])[b0:b0 + NB]
    ctx = np.asarray(inp['ctx'])[b0:b0 + NB]
    xT = np.ascontiguousarray(x.transpose(0, 2, 1).reshape(NB, 8, 128, cfg.N).transpose(0, 2, 1, 3).reshape(NB, 128, 8 * cfg.N))
    cT = np.ascontiguousarray(ctx.transpose(0, 2, 1).reshape(NB, 8, 128, cfg.NC).transpose(0, 2, 1, 3).reshape(NB, 128, 8 * cfg.NC))
    cv = np.zeros((3, D), np.float32)
    cv[:NB] = np.asarray(inp['c'])[b0:b0 + NB]
    cv[2] = np.asarray(inp['c_ctx'])
    cc = np.ascontiguousarray(cv.reshape(3, 8, 128).transpose(2, 1, 0).reshape(128, 24))
    return dict(xT=xT, ctxT=cT, cvec=cc)


DRAM_SHAPES = lambda cfg: dict(
    xT=[cfg.NB, 128, 8 * cfg.N], ctxT=[cfg.NB, 128, 8 * cfg.NC], cvec=[128, 24],
    wada=[cfg.L, 12, 128, 4096], bada=[128, cfg.L * 48], gvec=[128, cfg.L * 32],
    win=[cfg.L, NCH, 128, 1024], bin=[128, cfg.L * NCH], wtm=[cfg.L, NTM, 128, 1024], btm=[1, cfg.L * NTM * 128],
    wout=[cfg.L, 8, 128, 1280], wgu=[cfg.L, FC, 128, 2048], wd=[cfg.L, 8, 128, FC * 128],
    mlnorm=[128, cfg.L * 4], danorm=[128, cfg.L], hgnorm=[128, cfg.L * 2], hglb=[128, cfg.LTOT * 8],
    dalam=[1, cfg.L * 192], mlfb=[128, cfg.L],
    cst_bf=[128, 1280], cst_f=[128, 96 + 96 + 128 + 512 + 2], ropeC=[128, cfg.T], ropeS=[128, cfg.T])


def build(cfg, debug=None):
    nc = bass.Bass("TRN2", target_bir_lowering=False)
    kb = KB(nc)
    T, NT, N, NC_, L, NB = cfg.T, cfg.NT, cfg.N, cfg.NC, cfg.L, cfg.NB
    shapes = DRAM_SHAPES(cfg)
    dr = {k: nc.dram_tensor(k, v, F32, kind="ExternalInput") for k, v in shapes.items()}
    d_out = nc.dram_tensor("outT", [NB, 128, 8 * N], F32, kind="ExternalOutput")

    def dap(name, off, dims):
        return bass.AP(dr[name], off, [list(x) for x in dims])

    MIXD = kb.dr("mixd", 10 * 128, T, BF16)
    GSC = kb.dr("gsc", 24, T, F32)

    X = kb.sb("X", 128, 8 * T, F32)
    HT = kb.sb("HT", 128, max(8 * T, 4096 + FC * 512), BF16)
    CB = kb.sb("CB", 128, 1280, BF16)
    CF = kb.sb("CF", 128, shapes['cst_f'][1], F32)
    MOD = kb.sb("MOD", 128, L * 48 * 3, F32)
    PAR = kb.sb("PAR", 128, L * 4 * 24, F32)
    BADA = kb.sb("BADA", 128, L * 48, F32)
    GV = kb.sb("GV", 128, L * 32, F32)
    BIN = kb.sb("BIN", 128, L * NCH, F32)
    NBIN = kb.sb("NBIN", 128, L * NCH, F32)
    BTM = kb.sb("BTM", 1, NTM * 128, BF16)
    ONE1 = kb.sb("ONE1", 1, 128, BF16)
    SM = kb.sb("SM", 128, 320, F32)
    WS = [kb.sb(f"W{i}", 128, WSLOT, BF16) for i in range(4)]
    TF = [kb.sb(f"TF{i}", 128, 512, F32) for i in range(10)]
    TB = [kb.sb(f"TB{i}", 128, 512, BF16) for i in range(5)]
    QK = kb.sb("QK", 128, max(2 * T, 4096), BF16)

    class Sub:
        def __init__(s_, t, base):
            s_.t, s_.base = t, base

        def c(s_, p0, n, off, cnt_):
            return s_.t.c(p0, n, s_.base + off, cnt_)

        def v(s_, p0, n, off, *dims):
            return s_.t.v(p0, n, s_.base + off, *dims)
    QT = Sub(QK, 0)
    KT = Sub(QK, T)
    YSQ = Sub(QK, 0)
    VE = kb.sb("VE", 128, NT * 97, BF16)
    G3 = kb.sb("G3", 128, T, BF16)
    HF = kb.sb("HF", 128, max(T, 4096), F32)
    YB = HF
    SST = kb.sb("SST", 128, 2 * 128, F32)
    SSB = kb.sb("SSB", 128, 4 * 128, BF16)
    BANKS = [kb.ps(f"PB{i}", 128, 512, F32) for i in range(8)]

    bank_free = list(range(8))

    def bget():
        return bank_free.pop(0)

    def bput(b):
        bank_free.append(b)

    cnt = {"tf": 0, "tb": 0, "ws": 0, "ssb": 0}

    def tf():
        cnt["tf"] += 1
        return TF[cnt["tf"] % len(TF)]

    def tb():
        cnt["tb"] += 1
        return TB[cnt["tb"] % len(TB)]

    def wslot():
        cnt["ws"] += 1
        return WS[cnt["ws"] % len(WS)]

    IDENT = CB.c(0, 128, 0, 128)
    MSK = {"F": CB.c(0, 128, 128, 128), "R": CB.c(0, 128, 256, 128)}
    M32 = {"F": CB.c(0, 128, 384, 128), "R": CB.c(0, 128, 512, 128)}
    ONESD = CB.c(0, 128, 640, 128)
    E97 = CF.c(0, 97, 0, 96)
    ONES96 = CF.c(0, 96, 96, 96)
    BD64 = CF.c(0, 128, 192, 128)
    RST32 = lambda n: CF.c(0, 128, 320, n)
    C1 = CF.c(0, 8, 832, 1)
    C2 = CF.c(0, 8, 833, 1)

    kb.dma("pool", CB.c(0, 128, 0, 1280), dap("cst_bf", 0, [[1280, 128], [1, 1280]]))
    kb.dma("sp", CF.c(0, 128, 0, CF.F), dap("cst_f", 0, [[CF.F, 128], [1, CF.F]]))
    kb.dma("sp", BADA.c(0, 128, 0, L * 48), dap("bada", 0, [[L * 48, 128], [1, L * 48]]))
    kb.dma("sp", GV.c(0, 128, 0, L * 32), dap("gvec", 0, [[L * 32, 128], [1, L * 32]]))
    kb.dma("sp", BIN.c(0, 128, 0, L * NCH), dap("bin", 0, [[L * NCH, 128], [1, L * NCH]]))
    kb.memset(ONE1.c(0, 1, 0, 128), 1.0)
    kb.memset(VE.c(0, 128, 0, NT * 97), 1.0)
    o_ml, o_da, o_hg, o_lb, o_fb, o_cv = 0, 16, 20, 28, 60, 64
    kb.dma("sp", SM.c(0, 128, o_ml, L * 4), dap("mlnorm", 0, [[L * 4, 128], [1, L * 4]]))
    kb.dma("sp", SM.c(0, 128, o_da, L), dap("danorm", 0, [[L, 128], [1, L]]))
    kb.dma("sp", SM.c(0, 128, o_hg, L * 2), dap("hgnorm", 0, [[L * 2, 128], [1, L * 2]]))
    kb.dma("sp", SM.c(0, 128, o_lb, cfg.LTOT * 8), dap("hglb", 0, [[cfg.LTOT * 8, 128], [1, cfg.LTOT * 8]]))
    kb.dma("sp", SM.c(0, 128, o_fb, L), dap("mlfb", 0, [[L, 128], [1, L]]))
    kb.dma("sp", SM.c(0, 128, o_cv, 24), dap("cvec", 0, [[24, 128], [1, 24]]))
    for l in range(L):
        kb.tt(BIN.c(0, 8, l * NCH + 17, 1), BIN.c(0, 8, l * NCH + 17, 1), SM.c(0, 8, o_fb + l, 1), ALU.add)
    kb.ts(NBIN.c(0, 128, 0, L * NCH), BIN.c(0, 128, 0, L * NCH), -1.0, None, ALU.mult)
    o_dg = 88
    for l in range(L):
        lam_init = 0.8 - 0.6 * math.exp(-0.3 * (cfg.layer0 + l))
        kb.ts(SM.c(0, 96, o_dg + l, 1), SM.c(0, 96, o_da + l, 1), 1.0 - lam_init, None, ALU.mult)
    o_nl = 96
    o_s = 104
    for l in range(L):
        lam_init = 0.8 - 0.6 * math.exp(-0.3 * (cfg.layer0 + l))
        LAMB = tf()
        kb.dma("sp", LAMB.c(0, 128, 0, 192), dap("dalam", l * 192, [[0, 128], [1, 192]]))
        junk = tf()
        for j in range(2):
            kb.stt(junk.c(0, 128, 0, 48), LAMB.c(0, 128, 96 * j, 48), 1.0, LAMB.c(0, 128, 96 * j + 48, 48),
                   ALU.mult, ALU.mult, accum=SM.c(0, 128, o_s + j, 1))
        kb.act(SM.c(0, 128, o_s + 2, 2), SM.c(0, 128, o_s, 2), AF.Exp)
        kb.tt(SM.c(0, 128, o_s + 4, 1), SM.c(0, 128, o_s + 3, 1), SM.c(0, 128, o_s + 2, 1), ALU.subtract)
        kb.ts(SM.c(0, 128, o_nl + l, 1), SM.c(0, 128, o_s + 4, 1), -lam_init, None, ALU.add)
    o_LB, o_OML, o_LBM, o_e = 112, 144, 176, 208
    LT = cfg.LTOT
    mx = SM.c(0, 128, o_e + 40, 8)
    kb.copy(mx, SM.c(0, 128, o_lb, 8))
    for l in range(1, LT):
        kb.tt(mx, mx, SM.c(0, 128, o_lb + 8 * l, 8), ALU.max)
    for l in range(LT):
        kb.tt(SM.c(0, 128, o_e + 8 * l, 8), SM.c(0, 128, o_lb + 8 * l, 8), mx, ALU.subtract)
    kb.act(SM.c(0, 128, o_e, 8 * LT), SM.c(0, 128, o_e, 8 * LT), AF.Exp)
    sm_ = SM.c(0, 128, o_e + 32, 8)
    kb.copy(sm_, SM.c(0, 128, o_e, 8))
    for l in range(1, LT):
        kb.tt(sm_, sm_, SM.c(0, 128, o_e + 8 * l, 8), ALU.add)
    kb.recip(sm_, sm_)
    cum = SM.c(0, 128, o_e + 48, 8)
    kb.memset(cum, 0.0)
    for la in range(LT):
        if la >= 1:
            t_ = SM.c(0, 128, o_e + 8 * la, 8)
            kb.tt(t_, t_, sm_, ALU.mult)
            kb.tt(cum, cum, t_, ALU.add)
        l = la - cfg.layer0
        if 0 <= l < L:
            kb.copy(SM.c(0, 128, o_LB + 8 * l, 8), cum)
            kb.ts(SM.c(0, 128, o_OML + 8 * l, 8), cum, -1.0, 1.0, ALU.mult, ALU.add)
            kb.ts(SM.c(0, 128, o_LBM + 8 * l, 8), cum, -1.0, None, ALU.add)

    SCT = TB[0]
    e_ = tf()
    kb.act(e_.c(0, 128, 0, 24), SM.c(0, 128, o_cv, 24), AF.Exp, scale=-1.0)
    kb.ts(e_.c(0, 128, 0, 24), e_.c(0, 128, 0, 24), 1.0, None, ALU.add)
    kb.recip(e_.c(0, 128, 0, 24), e_.c(0, 128, 0, 24))
    kb.tt(SCT.c(0, 128, 0, 24), e_.c(0, 128, 0, 24), SM.c(0, 128, o_cv, 24), ALU.mult)
    for l in range(L):
        for grp in range(12):
            w0 = wslot()
            w1 = wslot()
            kb.dma("pool", w0.c(0, 128, 0, 2048), dap("wada", (l * 12 + grp) * 128 * 4096, [[4096, 128], [1, 2048]]))
            kb.dma("pool", w1.c(0, 128, 0, 2048), dap("wada", (l * 12 + grp) * 128 * 4096 + 2048, [[4096, 128], [1, 2048]]))
            b = bget()
            for c4 in range(4):
                w = w0 if c4 < 2 else w1
                for kc in range(8):
                    kb.mm(BANKS[b].c(0, 128, c4 * 3, 3), w.c(0, 128, (c4 % 2) * 1024 + kc * 128, 128), SCT.c(0, 128, kc * 3, 3),
                          start=(kc == 0), stop=(kc == 7))
            kb.tt(MOD.v(0, 128, (l * 48 + grp * 4) * 3, (3, 4), (1, 3)), BANKS[b].v(0, 128, 0, (3, 4), (1, 3)),
                  BADA.v(0, 128, l * 48 + grp * 4, (1, 4), (0, 3)), ALU.add)
            bput(b)
        modv = lambda part: MOD.v(0, 128, (l * 48 + part * 8) * 3, (3, 8), (1, 3))
        gvv = lambda k: GV.v(0, 128, l * 32 + k * 8, (1, 8), (0, 3))
        parv = lambda k: PAR.v(0, 128, (l * 4 + k) * 24, (3, 8), (1, 3))
        kb.stt(parv(0), modv(1), 1.0, gvv(0), ALU.add, ALU.mult)
        kb.tt(parv(1), modv(2), gvv(1), ALU.mult)
        kb.stt(parv(2), modv(4), 1.0, gvv(2), ALU.add, ALU.mult)
        kb.tt(parv(3), modv(5), gvv(3), ALU.mult)

    def par(l, k, kc, v):
        return PAR.c(0, 128, (l * 4 + k) * 24 + kc * 3 + v, 1)

    def modc(l, part, kc, v):
        return MOD.c(0, 128, (l * 48 + part * 8 + kc) * 3 + v, 1)

    def bias(l, ch, np_=128):
        return BIN.c(0, np_, l * NCH + ch, 1)

    def nbias(l, ch, np_=128):
        return NBIN.c(0, np_, l * NCH + ch, 1)

    def Xv(kc, t0, n):
        return X.c(0, 128, kc * T + t0, n)

    def HTv(kc, t0, n, p0=0, np_=128):
        return HT.c(p0, np_, kc * T + t0, n)

    def rms_rstd(src3, n):
        sq = tb_big(n)
        kb.act(sq, src3, AF.Square)
        b = bget()
        for kc in range(8):
            kb.mm(BANKS[b].c(0, 128, 0, n), ONESD, YSQ.c(0, 128, kc * 512, n), start=(kc == 0), stop=(kc == 7))
        r = tf()
        kb.act(r.c(0, 128, 0, n), BANKS[b].c(0, 128, 0, n), AF.Ln, bias=EPS)
        bput(b)
        kb.act(r.c(0, 128, 0, n), r.c(0, 128, 0, n), AF.Exp, scale=-0.5)
        return r

    LNS = math.log(96 ** -0.5)

    def tb_big(n):
        return YSQ.v(0, 128, 0, (512, 8), (1, n))

    def load_w(name, idx_off, ncols, row_stride):
        w = wslot()
        kb.dma("pool", w.c(0, 128, 0, ncols), dap(name, idx_off, [[row_stride, 128], [1, ncols]]))
        return w

    def load_fm(l, ch):
        return load_w("win", (l * NCH + ch) * 128 * 1024, 1024, 1024)

    def load_tm(l, i):
        return load_w("wtm", (l * NTM + i) * 128 * 1024, 1024, 1024)

    def proj_fm(w, t0, n, M, bank, col0=0, p0=0):
        for kc in range(8):
            kb.mm(BANKS[bank].c(p0, M, col0, n), w.c(0, 128, kc * 128, M), HTv(kc, t0, n), start=(kc == 0), stop=(kc == 7))

    def proj_tm(w, l, i, ncol, dst_of_tile):
        for tt0 in range(0, NT, 4):
            nt = min(4, NT - tt0)
            b = bget()
            for j in range(nt):
                tt = tt0 + j
                for kc in range(8):
                    kb.mm(BANKS[b].c(0, 128, j * 128, ncol), HTv(kc, tt * 128, 128), w.c(0, 128, kc * 128, ncol), start=(kc == 0), stop=False)
                kb.mm(BANKS[b].c(0, 128, j * 128, ncol), ONE1.c(0, 1, 0, 128), BTM.c(0, 1, i * 128, ncol), start=False, stop=True)
            kb.copy(dst_of_tile(tt0, nt), BANKS[b].v(0, 128, 0, (128, nt), (1, ncol)), eng="act")
            bput(b)

    def mixd_write(chunk, np_, t0, n, src):
        kb.dma("sp", MIXD.c(chunk * 128, np_, t0, n), src)

    def head_rstd(src, np_, n, avg):
        sq = tf()
        kb.act(sq.c(0, np_, 0, n), src, AF.Square)
        b = bget()
        kb.mm(BANKS[b].c(0, np_, 0, n), avg, sq.c(0, np_, 0, n))
        r = tf()
        kb.act(r.c(0, np_, 0, n), BANKS[b].c(0, np_, 0, n), AF.Ln, bias=EPS)
        bput(b)
        kb.act(r.c(0, np_, 0, n), r.c(0, np_, 0, n), AF.Exp, scale=-0.5)
        return r

    def den_bcast(src97, n):
        b = bget()
        kb.mm(BANKS[b].c(0, 96, 0, n), E97, src97)
        return b

    TG = [kb.sb(f"TG{i}", 128, 512, BF16) for i in range(4)]
    cnt["tg"] = 0
    KBD = [kb.sb(f"KBD{i}", 128, 512, BF16) for i in range(2)]
    cnt["kbd"] = 0
    for t_ in KBD:
        kb.memset(t_.c(0, 128, 0, 512), 0.0)

    def tg():
        cnt["tg"] += 1
        return TG[cnt["tg"] % len(TG)]

    def gsc_bcast(row, t0, n, np_=96):
        ap = bass.AP(GSC.h, row * T + t0, [[0, np_], [1, n]])
        return View(GSC, ap, row, row + 1, t0, t0 + n)

    groups = cfg.groups
    ctx_groups = [g for g in groups if g[2]]
    lat_groups = [g for g in groups if not g[2]]

    def scan_order(d):
        if d == 0:
            return [(g, list(range(g[1] // 128))) for g in groups]
        return [(g, list(range(g[1] // 128))[::-1]) for g in (ctx_groups[::-1] + lat_groups[::-1])]

    def phase_h(l, b, kA, sPart, dst, only=None):
        for (t0, n, isctx) in (groups if only is None else [only]):
            v = 2 if isctx else b
            r = rms_rstd(X.v(0, 128, t0, (T, 8), (1, n)), n)
            for kc in range(8):
                tmp = tf()
                kb.tt(tmp.c(0, 128, 0, n), Xv(kc, t0, n), r.c(0, 128, 0, n), ALU.mult)
                kb.act(dst(kc, t0, n), tmp.c(0, 128, 0, n), AF.Identity, bias=modc(l, sPart, kc, v), scale=par(l, kA, kc, v))

    def resid_update(l, b, kG, t0, n, v):
        r = rms_rstd(YB.v(0, 128, 0, (512, 8), (1, n)), n)
        for dc in range(8):
            tmp = tf()
            kb.tt(tmp.c(0, 128, 0, n), YB.c(0, 128, dc * 512, n), r.c(0, 128, 0, n), ALU.mult)
            kb.stt(Xv(dc, t0, n), tmp.c(0, 128, 0, n), par(l, kG, dc, v), Xv(dc, t0, n), ALU.mult, ALU.add)

    def sigmoid_from_psum(dst, bank, np_, n, nb):
        e = tf()
        kb.act(e.c(0, np_, 0, n), BANKS[bank].c(0, np_, 0, n), AF.Exp, bias=nb, scale=-1.0)
        kb.act(e.c(0, np_, 0, n), e.c(0, np_, 0, n), AF.Ln, bias=1.0)
        kb.act(dst, e.c(0, np_, 0, n), AF.Exp, scale=-1.0)

    def silu_from_psum(dst, bank, np_, n, bcol, nbcol):
        sg = tf()
        sigmoid_from_psum(sg.c(0, np_, 0, n), bank, np_, n, nbcol)
        kb.stt(dst, BANKS[bank].c(0, np_, 0, n), bcol, sg.c(0, np_, 0, n), ALU.add, ALU.mult)

    class _Stop(Exception):
        pass

    def stop_if(tag):
        if debug == tag:
            raise _Stop()

    for b in range(NB):
      try:
        kb.dma("sp", X.v(0, 128, NC_, (T, 8), (1, N)), bass.AP(dr["xT"], b * 128 * 8 * N, [[8 * N, 128], [N, 8], [1, N]]))
        kb.dma("sp", X.v(0, 128, 0, (T, 8), (1, NC_)), bass.AP(dr["ctxT"], b * 128 * 8 * NC_, [[8 * NC_, 128], [NC_, 8], [1, NC_]]))
        for l in range(L):
            last = (cfg.layer0 + l == cfg.LTOT - 1)
            kb.dma("pool", BTM.c(0, 1, 0, NTM * 128), dap("btm", l * NTM * 128, [[NTM * 128, 1], [1, NTM * 128]]))
            phase_h(l, b, 0, 0, HTv)
            if debug == "ht" and b == 0 and l == 0:
                break
            for h in range(4):
                for (c0, dst) in ((4 * h, QT), (4 * h + 2, KT)):
                    w1 = load_fm(l, c0)
                    w2 = load_fm(l, c0 + 1)
                    for (t0, n, _) in groups:
                        b1 = bget()
                        b2 = bget()
                        proj_fm(w1, t0, n, 128, b1)
                        proj_fm(w2, t0, n, 128, b2)
                        t1 = tf()
                        t2 = tf()
                        rc = tb()
                        kb.dma("pool", rc.c(0, 128, 0, n), dap("ropeC", t0, [[T, 128], [1, n]]))
                        rs_ = tb()
                        kb.dma("pool", rs_.c(0, 128, 0, n), dap("ropeS", t0, [[T, 128], [1, n]]))
                        kb.stt(t1.c(0, 128, 0, n), BANKS[b1].c(0, 128, 0, n), bias(l, c0), rc.c(0, 128, 0, n), ALU.add, ALU.mult)
                        kb.stt(t2.c(0, 128, 0, n), BANKS[b2].c(0, 128, 0, n), bias(l, c0 + 1), rs_.c(0, 128, 0, n), ALU.add, ALU.mult)
                        bput(b1)
                        bput(b2)
                        kb.tt(dst.c(0, 128, t0, n), t1.c(0, 128, 0, n), t2.c(0, 128, 0, n), ALU.add)
                wv = load_tm(l, 8 + h)
                proj_tm(wv, l, 8 + h, 96, lambda tt0, nt: VE.v(0, 128, tt0 * 97, (97, nt), (1, 96)))
                sc_ = 48 ** -0.5
                for (q0, n, isctx) in groups:
                    if isctx and last:
                        continue
                    kts = list(range(NC_ // 128)) if isctx else list(range(NT))
                    O = [bget(), bget()]
                    for i, kt in enumerate(kts):
                        for m in range(2):
                            sb_ = bget()
                            kb.mm(BANKS[sb_].c(0, 128, 0, n), KT.c(64 * m, 64, kt * 128, 128), QT.c(64 * m, 64, q0, n))
                            p = tb()
                            kb.act(p.c(0, 128, 0, n), BANKS[sb_].c(0, 128, 0, n), AF.Exp, scale=sc_)
                            bput(sb_)
                            kb.mm(BANKS[O[m]].c(0, 97, 0, n), VE.c(0, 128, kt * 97, 97), p.c(0, 128, 0, n),
                                  start=(i == 0), stop=(i == len(kts) - 1))
                    osb = [tf(), tf()]
                    for m in range(2):
                        kb.copy(osb[m].c(0, 97, 0, n), BANKS[O[m]].c(0, 97, 0, n), eng="act")
                        bput(O[m])
                    rr = []
                    for m in range(2):
                        db = den_bcast(osb[m].c(0, 97, 0, n), n)
                        rm = tf()
                        kb.act(rm.c(0, 96, 0, n), BANKS[db].c(0, 96, 0, n), AF.Ln)
                        bput(db)
                        kb.act(rm.c(0, 96, 0, n), rm.c(0, 96, 0, n), AF.Exp, scale=-1.0)
                        rr.append(rm)
                    a_ = tf()
                    kb.tt(a_.c(0, 96, 0, n), osb[0].c(0, 96, 0, n), rr[0].c(0, 96, 0, n), ALU.mult)
                    b_ = tf()
                    kb.tt(b_.c(0, 96, 0, n), osb[1].c(0, 96, 0, n), rr[1].c(0, 96, 0, n), ALU.mult)
                    o_ = tf()
                    kb.stt(o_.c(0, 96, 0, n), b_.c(0, 96, 0, n), SM.c(0, 96, o_nl + l, 1), a_.c(0, 96, 0, n), ALU.mult, ALU.add)
                    rs = head_rstd(o_.c(0, 96, 0, n), 96, n, ONES96)
                    ob = tb()
                    kb.stt(ob.c(0, 96, 0, n), o_.c(0, 96, 0, n), SM.c(0, 96, o_dg + l, 1), rs.c(0, 96, 0, n), ALU.mult, ALU.mult)
                    mixd_write(6 + h, 96, q0, n, ob.c(0, 96, 0, n))
            if debug == "da":
                break
            wi = load_fm(l, 16)
            wf = load_fm(l, 17)
            for (t0, n, _) in groups:
                nch = n // 128
                bi = bget()
                bf_ = bget()
                proj_fm(wi, t0, n, 8, bi)
                proj_fm(wf, t0, n, 8, bf_)
                GI = tf()
                kb.act(GI.c(0, 8, 0, n), BANKS[bi].c(0, 8, 0, n), AF.Identity, bias=bias(l, 16, 8))
                bput(bi)
                e = tf()
                kb.act(e.c(0, 8, 0, n), BANKS[bf_].c(0, 8, 0, n), AF.Exp, bias=nbias(l, 17, 8), scale=-1.0)
                bput(bf_)
                NLF = tf()
                kb.act(NLF.c(0, 8, 0, n), e.c(0, 8, 0, n), AF.Ln, bias=1.0)
                CUM = tf()
                for ci in range(nch):
                    kb.scan(CUM.c(0, 8, ci * 128, 128), CB.c(0, 8, 768, 128), NLF.c(0, 8, ci * 128, 128), 0.0, ALU.mult, ALU.add)
                v3 = lambda t_: t_.v(0, 8, 0, (128, nch), (1, 128))
                TOTv = CUM.v(0, 8, 127, (128, nch), (0, 128))
                U = tf()
                kb.tt(v3(U), TOTv, v3(NLF), ALU.add)
                kb.ts(U.c(0, 8, 0, n), U.c(0, 8, 0, n), C2, None, ALU.mult)
                NBt = tf()
                kb.stt(NBt.c(0, 8, 0, n), CUM.c(0, 8, 0, n), C1, U.c(0, 8, 0, n), ALU.mult, ALU.add)
                EQ = tf()
                kb.act(EQ.c(0, 8, 0, n), NBt.c(0, 8, 0, n), AF.Exp, scale=-1.0)
                Wt = tf()
                kb.tt(Wt.c(0, 8, 0, n), GI.c(0, 8, 0, n), NBt.c(0, 8, 0, n), ALU.add)
                EK = tf()
                kb.act(EK.c(0, 8, 0, n), Wt.c(0, 8, 0, n), AF.Exp, bias=LNS)
                W2 = tf()
                kb.tt(v3(W2), v3(Wt), TOTv, ALU.subtract)
                EKH = tf()
                kb.act(EKH.c(0, 8, 0, n), W2.c(0, 8, 0, n), AF.Exp, bias=LNS)
                kb.dma("sp", GSC.c(0, 8, t0, n), EQ.c(0, 8, 0, n))
                kb.dma("sp", GSC.c(8, 8, t0, n), EK.c(0, 8, 0, n))
                kb.dma("sp", GSC.c(16, 8, t0, n), EKH.c(0, 8, 0, n))
            if debug == "ml_g":
                break
            for h in range(4):
                for (ch, dst) in ((18 + 3 * h, QT), (19 + 3 * h, KT)):
                    w = load_fm(l, ch)
                    for (t0, n, _) in groups:
                        bk = bget()
                        proj_fm(w, t0, n, 96, bk)
                        kb.act(dst.c(0, 96, t0, n), BANKS[bk].c(0, 96, 0, n), AF.Identity, bias=bias(l, ch, 96))
                        bput(bk)
                w = load_fm(l, 20 + 3 * h)
                for (t0, n, _) in groups:
                    bk = bget()
                    proj_fm(w, t0, n, 96, bk)
                    sigmoid_from_psum(G3.c(0, 96, t0, n), bk, 96, n, nbias(l, 20 + 3 * h, 96))
                    bput(bk)
                wv = load_tm(l, h)
                proj_tm(wv, l, h, 96, lambda tt0, nt: VE.v(0, 128, tt0 * 97, (97, nt), (1, 96)))
                if debug == "ml_p":
                    break
                for d in range(2):
                    g = d * 4 + h
                    if debug == "ml_f" and d == 1:
                        break
                    dk = "FR"[d]
                    have_state = False
                    sslot = 0
                    SSc = SST.c(0, 96, 0, 97)
                    for ((t0, n, isctx), tiles) in scan_order(d):
                        BQ = tf()
                        kb.dma("sp", BQ.c(0, 96, 0, n), gsc_bcast(g, t0, n))
                        BK = tf()
                        kb.dma("sp", BK.c(0, 96, 0, n), gsc_bcast(8 + g, t0, n))
                        BKH = tf()
                        kb.dma("sp", BKH.c(0, 96, 0, n), gsc_bcast(16 + g, t0, n))
                        qs = tg()
                        kb.tt(qs.c(0, 96, 0, n), QT.c(0, 96, t0, n), BQ.c(0, 96, 0, n), ALU.mult)
                        ks = tg()
                        kb.tt(ks.c(0, 96, 0, n), KT.c(0, 96, t0, n), BK.c(0, 96, 0, n), ALU.mult)
                        kh = tg()
                        kb.tt(kh.c(0, 96, 0, n), KT.c(0, 96, t0, n), BKH.c(0, 96, 0, n), ALU.mult)
                        Hb = bget()
                        for j in tiles:
                            c = t0 // 128 + j
                            cols = j * 128
                            sb_ = bget()
                            kb.mm(BANKS[sb_].c(0, 128, 0, 128), ks.c(0, 96, cols, 128), qs.c(0, 96, cols, 128))
                            at = tb()
                            kb.tt(at.c(0, 128, 0, 128), BANKS[sb_].c(0, 128, 0, 128), MSK[dk], ALU.mult)
                            bput(sb_)
                            kb.mm(BANKS[Hb].c(0, 97, cols, 128), VE.c(0, 128, c * 97, 97), at.c(0, 128, 0, 128),
                                  start=True, stop=not have_state)
                            if have_state:
                                kb.mm(BANKS[Hb].c(0, 97, cols, 128), SSB.c(0, 96, sslot * 128, 97), qs.c(0, 96, cols, 128),
                                      start=False, stop=True)
                            tb_ = bget()
                            kb.mm(BANKS[tb_].c(0, 128, 0, 96), kh.c(0, 96, cols, 128), CB.c(0, 96, 0, 96))
                            ktok = tb()
                            kb.copy(ktok.c(0, 128, 0, 96), BANKS[tb_].c(0, 128, 0, 96), eng="act")
                            bput(tb_)
                            db_ = bget()
                            kb.mm(BANKS[db_].c(0, 96, 0, 97), ktok.c(0, 128, 0, 96), VE.c(0, 128, c * 97, 97))
                            eB = BQ.c(0, 96, cols + (127 if d == 0 else 0), 1)
                            if not have_state:
                                kb.copy(SSc, BANKS[db_].c(0, 96, 0, 97))
                            else:
                                kb.stt(SSc, SSc, eB, BANKS[db_].c(0, 96, 0, 97), ALU.mult, ALU.add)
                            bput(db_)
                            have_state = True
                            sslot = (sslot + 1) % 4
                            kb.copy(SSB.c(0, 96, sslot * 128, 97), SSc, eng="act")
                        hsb = tf()
                        kb.copy(hsb.c(0, 97, 0, n), BANKS[Hb].c(0, 97, 0, n), eng="act")
                        bput(Hb)
                        db = den_bcast(hsb.c(0, 97, 0, n), n)
                        dn = tf()
                        kb.act(dn.c(0, 96, 0, n), BANKS[db].c(0, 96, 0, n), AF.Abs)
                        bput(db)
                        kb.ts(dn.c(0, 96, 0, n), dn.c(0, 96, 0, n), 1.0, None, ALU.max)
                        kb.act(dn.c(0, 96, 0, n), dn.c(0, 96, 0, n), AF.Ln)
                        kb.act(dn.c(0, 96, 0, n), dn.c(0, 96, 0, n), AF.Exp, scale=-1.0)
                        if d == 0 or debug == "ml_r":
                            kb.tt(HF.c(0, 96, t0, n), hsb.c(0, 96, 0, n), dn.c(0, 96, 0, n), ALU.mult)
                        else:
                            hs = tf()
                            kb.tt(hs.c(0, 96, 0, n), hsb.c(0, 96, 0, n), dn.c(0, 96, 0, n), ALU.mult)
                            kb.tt(hs.c(0, 96, 0, n), hs.c(0, 96, 0, n), HF.c(0, 96, t0, n), ALU.add)
                            rs = head_rstd(hs.c(0, 96, 0, n), 96, n, ONES96)
                            t3 = tf()
                            kb.stt(t3.c(0, 96, 0, n), hs.c(0, 96, 0, n), SM.c(0, 96, o_ml + l * 4 + h, 1), rs.c(0, 96, 0, n),
                                   ALU.mult, ALU.mult)
                            ob = tb()
                            kb.tt(ob.c(0, 96, 0, n), t3.c(0, 96, 0, n), G3.c(0, 96, t0, n), ALU.mult)
                            mixd_write(h, 96, t0, n, ob.c(0, 96, 0, n))
            if debug in ("ml", "ml_p", "ml_f", "ml_r"):
                break
            for p in range(2):
                w = load_fm(l, 42 + p)
                for (t0, n, _) in groups:
                    bk = bget()
                    proj_fm(w, t0, n, 128, bk)
                    silu_from_psum(G3.c(0, 128, t0, n), bk, 128, n, bias(l, 42 + p), nbias(l, 42 + p))
                    bput(bk)
                for hh in range(2):
                    h = 2 * p + hh
                    w = load_fm(l, 30 + 3 * h)
                    for (t0, n, _) in groups:
                        bk = bget()
                        proj_fm(w, t0, n, 128, bk)
                        silu_from_psum(QT.c(0, 128, t0, n), bk, 128, n, bias(l, 30 + 3 * h), nbias(l, 30 + 3 * h))
                        bput(bk)
                    wv = load_tm(l, 4 + h)
                    proj_tm(wv, l, 4 + h, 64, lambda tt0, nt: VE.v(0, 128, tt0 * 97, (97, nt), (1, 64)))
                    stop_if("hg_p")
                    for d in range(2):
                        dk = "FR"[d]
                        chz = 31 + 3 * h + d
                        wz = load_fm(l, chz)
                        gi = l * 8 + d * 4 + h
                        lbc, omlc, lbmc = SM.c(0, 128, o_LB + gi, 1), SM.c(0, 128, o_OML + gi, 1), SM.c(0, 128, o_LBM + gi, 1)
                        have_state = False
                        sslot = 0
                        SSc = SST.c(0, 128, 128, 64)
                        for ((t0, n, isctx), tiles) in scan_order(d):
                            nsub = n // 32
                            f2 = lambda t_: t_.c(0, 128, 0, n)
                            f3 = lambda t_: t_.v(0, 128, 0, (32, nsub), (1, 32))
                            zb = bget()
                            proj_fm(wz, t0, n, 128, zb)
                            sg = tf()
                            sigmoid_from_psum(f2(sg), zb, 128, n, nbias(l, chz))
                            bput(zb)
                            fg = tf()
                            kb.ts(f2(fg), f2(sg), omlc, lbc, ALU.mult, ALU.add)
                            kk = tf()
                            kb.ts(f2(kk), f2(sg), lbmc, omlc, ALU.mult, ALU.add)
                            lf = tf()
                            kb.act(f2(lf), f2(fg), AF.Ln)
                            cum = tf()
                            kb.scan(f2(cum), RST32(n), f2(lf), 0.0, ALU.mult, ALU.add)
                            TOTv = cum.v(0, 128, 31, (32, nsub), (0, 32))
                            dd = tf()
                            if d == 0:
                                bc = cum
                                kb.tt(f3(dd), TOTv, f3(cum), ALU.subtract)
                            else:
                                u = tf()
                                kb.tt(f3(u), TOTv, f3(lf), ALU.add)
                                bc = tf()
                                kb.tt(f2(bc), f2(u), f2(cum), ALU.subtract)
                                kb.tt(f2(dd), f2(cum), f2(lf), ALU.subtract)
                            eb = tf()
                            kb.act(f2(eb), f2(bc), AF.Exp)
                            qs = tg()
                            kb.tt(f2(qs), QT.c(0, 128, t0, n), f2(eb), ALU.mult)
                            enb = tf()
                            kb.act(f2(enb), f2(bc), AF.Exp, scale=-1.0)
                            ks = tg()
                            kb.tt(f2(ks), f2(kk), f2(enb), ALU.mult)
                            ed = tf()
                            kb.act(f2(ed), f2(dd), AF.Exp)
                            kh = tg()
                            kb.tt(f2(kh), f2(kk), f2(ed), ALU.mult)
                            stop_if("hg_e")
                            Ob = bget()
                            for j in tiles:
                                c = t0 // 128 + j
                                cols = j * 128
                                sb_ = bget()
                                kb.mm(BANKS[sb_].c(0, 128, 0, 128), ks.c(0, 128, cols, 128), qs.c(0, 128, cols, 128))
                                at = tb()
                                kb.tt(at.c(0, 128, 0, 128), BANKS[sb_].c(0, 128, 0, 128), M32[dk], ALU.mult)
                                bput(sb_)
                                kb.mm(BANKS[Ob].c(hh * 64, 64, cols, 128), VE.c(0, 128, c * 97, 64), at.c(0, 128, 0, 128),
                                      start=True, stop=True, skip_group_check=True)
                                tb_ = bget()
                                kb.mm(BANKS[tb_].c(0, 128, 0, 128), kh.c(0, 128, cols, 128), IDENT)
                                cnt["kbd"] += 1
                                kbd = KBD[cnt["kbd"] % 2]
                                kb.tt(kbd.v(0, 128, 0, (128, 4), (1, 128)), BANKS[tb_].v(0, 128, 0, (0, 4), (1, 128)),
                                      CB.v(0, 128, 768, (128, 4), (1, 128)), ALU.mult)
                                bput(tb_)
                                db_ = bget()
                                for sc in range(4):
                                    kb.mm(BANKS[db_].c(0, 128, sc * 64, 64), kbd.c(0, 128, sc * 128, 128), VE.c(0, 128, c * 97, 64))
                                stop_if("hg_t")
                                subs = list(range(4)) if d == 0 else list(range(4))[::-1]
                                for sc in subs:
                                    scol = cols + sc * 32
                                    if have_state:
                                        kb.mm(BANKS[Ob].c(hh * 64, 64, scol, 32), SSB.c(0, 128, sslot * 128, 64), qs.c(0, 128, scol, 32),
                                              start=False, stop=True, skip_group_check=True)
                                    eL = eb.c(0, 128, scol + (31 if d == 0 else 0), 1)
                                    if not have_state:
                                        kb.copy(SSc, BANKS[db_].c(0, 128, sc * 64, 64))
                                    else:
                                        kb.stt(SSc, SSc, eL, BANKS[db_].c(0, 128, sc * 64, 64), ALU.mult, ALU.add)
                                    have_state = True
                                    sslot = (sslot + 1) % 4
                                    kb.copy(SSB.c(0, 128, sslot * 128, 64), SSc, eng="act")
                                bput(db_)
                            hfv = HF.c(hh * 64, 64, t0, n)
                            if d == 0:
                                kb.copy(hfv, BANKS[Ob].c(hh * 64, 64, 0, n), eng="act")
                            else:
                                kb.tt(hfv, hfv, BANKS[Ob].c(hh * 64, 64, 0, n), ALU.add)
                            bput(Ob)
                for (t0, n, _) in groups:
                    rs = head_rstd(HF.c(0, 128, t0, n), 128, n, BD64)
                    t3 = tf()
                    kb.stt(t3.c(0, 128, 0, n), HF.c(0, 128, t0, n), SM.c(0, 128, o_hg + l * 2 + p, 1), rs.c(0, 128, 0, n),
                           ALU.mult, ALU.mult)
                    ob = tb()
                    kb.tt(ob.c(0, 128, 0, n), t3.c(0, 128, 0, n), G3.c(0, 128, t0, n), ALU.mult)
                    mixd_write(4 + p, 128, t0, n, ob.c(0, 128, 0, n))
            if debug == "mix" and b == 0 and l == 0:
                break
            for (t0, n, isctx) in groups:
                if isctx and last:
                    continue
                v = 2 if isctx else b
                mv_ap = bass.AP(MIXD.h, t0, [[T, 128], [128 * T, 10], [1, n]])
                kb.dma("sp", HT.v(0, 128, 0, (512, 10), (1, n)), View(MIXD, mv_ap, 0, 1280, t0, t0 + n))
                for dc in range(8):
                    wo = load_w("wout", (l * 8 + dc) * 128 * 1280, 1280, 1280)
                    yb = bget()
                    for kc in range(10):
                        K = 128 if kc in (4, 5) else 96
                        kb.mm(BANKS[yb].c(0, 128, 0, n), wo.c(0, K, kc * 128, 128), HT.c(0, K, kc * 512, n),
                              start=(kc == 0), stop=(kc == 9))
                    kb.copy(YB.c(0, 128, dc * 512, n), BANKS[yb].c(0, 128, 0, n), eng="act")
                    bput(yb)
                resid_update(l, b, 1, t0, n, v)
            H2 = lambda kc, t0, n: HT.c(0, 128, kc * 512, n)
            AB = lambda fc, n: HT.c(0, 128, 4096 + fc * 512, n)
            for grp in groups:
                (t0, n, isctx) = grp
                if isctx and last:
                    continue
                v = 2 if isctx else b
                phase_h(l, b, 2, 3, H2, only=grp)
                for fc in range(FC):
                    w = load_w("wgu", (l * FC + fc) * 128 * 2048, 2048, 2048)
                    gb = bget()
                    ub = bget()
                    for kc in range(8):
                        kb.mm(BANKS[gb].c(0, 128, 0, n), w.c(0, 128, kc * 128, 128), H2(kc, t0, n), start=(kc == 0), stop=(kc == 7))
                    for kc in range(8):
                        kb.mm(BANKS[ub].c(0, 128, 0, n), w.c(0, 128, 1024 + kc * 128, 128), H2(kc, t0, n), start=(kc == 0), stop=(kc == 7))
                    e = tf()
                    kb.act(e.c(0, 128, 0, n), BANKS[gb].c(0, 128, 0, n), AF.Exp, scale=-1.0)
                    kb.act(e.c(0, 128, 0, n), e.c(0, 128, 0, n), AF.Ln, bias=1.0)
                    kb.act(e.c(0, 128, 0, n), e.c(0, 128, 0, n), AF.Exp, scale=-1.0)
                    t_ = tf()
                    kb.tt(t_.c(0, 128, 0, n), e.c(0, 128, 0, n), BANKS[gb].c(0, 128, 0, n), ALU.mult)
                    bput(gb)
                    kb.tt(AB(fc, n), t_.c(0, 128, 0, n), BANKS[ub].c(0, 128, 0, n), ALU.mult)
                    bput(ub)
                for dc in range(8):
                    wa = load_w("wd", (l * 8 + dc) * 128 * 2816, 1408, 2816)
                    wb = load_w("wd", (l * 8 + dc) * 128 * 2816 + 1408, 1408, 2816)
                    yb = bget()
                    for fc in range(FC):
                        w = wa if fc < 11 else wb
                        kb.mm(BANKS[yb].c(0, 128, 0, n), w.c(0, 128, (fc % 11) * 128, 128), AB(fc, n), start=(fc == 0), stop=(fc == FC - 1))
                    kb.copy(YB.c(0, 128, dc * 512, n), BANKS[yb].c(0, 128, 0, n), eng="act")
                    bput(yb)
                resid_update(l, b, 3, t0, n, v)
        if debug is not None:
            break
      except _Stop:
        break
      if True:
        kb.dma("sp", bass.AP(d_out, b * 128 * 8 * N, [[8 * N, 128], [N, 8], [1, N]]), X.v(0, 128, NC_, (T, 8), (1, N)), is_output=True)

    if debug is not None:
        dbg = nc.dram_tensor("dbg", [1280, T], F32, kind="ExternalOutput")
        if debug == "ht":
            for kc in range(8):
                for (t0, n, _) in groups:
                    t_ = tf()
                    kb.copy(t_.c(0, 128, 0, n), HTv(kc, t0, n))
                    kb.dma("sp", bass.AP(dbg, kc * 128 * T + t0, [[T, 128], [1, n]]), t_.c(0, 128, 0, n), is_output=True)
        else:
            for kc in range(10):
                for (t0, n, _) in groups:
                    s_ = tb()
                    kb.dma("sp", s_.c(0, 128, 0, n), MIXD.c(kc * 128, 128, t0, n))
                    t_ = tf()
                    kb.copy(t_.c(0, 128, 0, n), s_.c(0, 128, 0, n))
                    kb.dma("sp", bass.AP(dbg, kc * 128 * T + t0, [[T, 128], [1, n]]), t_.c(0, 128, 0, n), is_output=True)
        kb.dma("sp", bass.AP(d_out, 0, [[8 * N, 128], [N, 8], [1, N]]), X.v(0, 128, NC_, (T, 8), (1, N)), is_output=True)
    kb.finish()
    return nc, kb


def kernel(**inputs):
    cfg = Cfg()
    consts = host_consts(cfg)
    wts = prep_weights(inputs, cfg)
    nc, kb = build(cfg)
    in_maps = []
    for core in range(8):
        m = dict(consts)
        m.update(wts)
        m.update(prep_core(inputs, cfg, core * cfg.NB))
        in_maps.append(m)
    res = run_bass_kernel_spmd(nc, in_maps, core_ids=list(range(8)))
    out = np.empty((16, cfg.N, D), np.float32)
    for core in range(8):
        o = res.results[core]["outT"].reshape(cfg.NB, 128, 8, cfg.N)
        out[core * cfg.NB:(core + 1) * cfg.NB] = o.transpose(0, 3, 2, 1).reshape(cfg.NB, cfg.N, D)
    return out
```
